# Optimizing a Trainium2 kernel written in Bass

```python
import jax
import jax.numpy as jnp
from jax import lax
import numpy as np

D_MODEL = 1024
BATCH = 2
SEQ = 16384
DEPTH = 4

GRID_W = 64
CTX_LEN = 256
EPS = 1e-6

LRU_WIDTH = D_MODEL
LRU_HEADS = 8
LRU_HEAD_DIM = LRU_WIDTH // LRU_HEADS
LRU_C = 8.0
CONV_W = 4
CONV_LEFT = 2
FNET_WIDTH = D_MODEL
FNET_GROUPS = 4
FNET_GROUP_DIM = FNET_WIDTH // FNET_GROUPS
EVEN_IN = 2 * (LRU_WIDTH + FNET_WIDTH)
EVEN_MIX = LRU_WIDTH + FNET_WIDTH

RET_HEADS = 4
RET_QK_DIM = D_MODEL // RET_HEADS
RET_V_DIM = 2 * D_MODEL // RET_HEADS
RET_QK = RET_HEADS * RET_QK_DIM
RET_MIX = RET_HEADS * RET_V_DIM
ODD_IN = 2 * RET_QK + 2 * RET_MIX
CHUNK = 128
ROPE_THETA = 10000.0
ROPE_FREQS = RET_QK_DIM // 4

kernel_name = 'hybrid_rglru_fourier_retention_prefix_trunk'


def rms_norm(x, g):
    xf = x.astype(jnp.float32)
    y = xf * lax.rsqrt(jnp.mean(xf * xf, axis=-1, keepdims=True) + EPS)
    return (y * g.astype(jnp.float32)).astype(x.dtype)


def ada_mod(cond, w, b):
    m = jax.nn.silu(cond) @ w + b
    return jnp.split(m, 3, axis=-1)


def dwconv(z, w, b):
    T = z.shape[1]
    zp = jnp.pad(z, ((0, 0), (CONV_LEFT, CONV_W - 1 - CONV_LEFT), (0, 0)))
    out = b + zp[:, 0:T] * w[0]
    for tap in range(1, CONV_W):
        out = out + zp[:, tap:tap + T] * w[tap]
    return out


def lru_coeffs(z, wa, ba, wx, bx, lam):
    B_, T, W = z.shape
    zf = z.astype(jnp.float32)
    zh = zf.reshape(B_, T, LRU_HEADS, LRU_HEAD_DIM)
    r = jax.nn.sigmoid(jnp.einsum('bthi,hij->bthj', zh, wa.astype(jnp.float32)).reshape(B_, T, W) + ba.astype(jnp.float32))
    i = jax.nn.sigmoid(jnp.einsum('bthi,hij->bthj', zh, wx.astype(jnp.float32)).reshape(B_, T, W) + bx.astype(jnp.float32))
    log_a = -LRU_C * r * jax.nn.softplus(-lam.astype(jnp.float32))
    a = jnp.exp(log_a)
    bterm = jnp.sqrt(-jnp.expm1(2.0 * log_a)) * (i * zf)
    return a, bterm


def _lin_combine(e1, e2):
    a1, b1 = e1
    a2, b2 = e2
    return a1 * a2, a2 * b1 + b2


def linear_scan(a, bterm, h0, reverse):
    A, H = lax.associative_scan(_lin_combine, (a, bterm), axis=1, reverse=reverse)
    if h0 is None:
        return H
    return H + A * h0[:, None, :]


def rglru_branch(xl, xc, conv_w, conv_b, wa, ba, wx, bx, lam):
    zl = dwconv(xl, conv_w, conv_b)
    zc = dwconv(xc, conv_w, conv_b)
    yl = None
    yc = None
    for d in range(2):
        rev = d == 1
        ac, bc = lru_coeffs(zc, wa[d], ba[d], wx[d], bx[d], lam[d])
        hc = linear_scan(ac, bc, None, rev)
        h_end = hc[:, 0] if rev else hc[:, -1]
        al, bl = lru_coeffs(zl, wa[d], ba[d], wx[d], bx[d], lam[d])
        hl = linear_scan(al, bl, h_end, rev)
        yl = hl if yl is None else yl + hl
        yc = hc if yc is None else yc + hc
    return yl.astype(xl.dtype), yc.astype(xc.dtype)


def fourier_branch(z, w_f):
    B_, T, W = z.shape
    zg = z.astype(jnp.float32).reshape(B_, T, FNET_GROUPS, FNET_GROUP_DIM)
    f = jnp.real(jnp.fft.fft2(zg, axes=(1, 3), norm='ortho'))
    y = jnp.einsum('btgi,gij->btgj', f, w_f.astype(jnp.float32))
    return y.reshape(B_, T, W).astype(z.dtype)


def even_mixer(h, hc, w_in, w_out, conv_w, conv_b, wa, ba, wx, bx, lam, w_f, need_ctx):
    cuts = [LRU_WIDTH, LRU_WIDTH + FNET_WIDTH, 2 * LRU_WIDTH + FNET_WIDTH]
    xa, xb, ga, gb = jnp.split(h @ w_in, cuts, axis=-1)
    xca, xcb, gca, gcb = jnp.split(hc @ w_in, cuts, axis=-1)
    ya, yca = rglru_branch(xa, xca, conv_w, conv_b, wa, ba, wx, bx, lam)
    yb = fourier_branch(xb, w_f)
    y = jnp.concatenate([ya * jax.nn.silu(ga), yb * jax.nn.silu(gb)], axis=-1) @ w_out
    if not need_ctx:
        return y, None
    ycb = fourier_branch(xcb, w_f)
    yc = jnp.concatenate([yca * jax.nn.silu(gca), ycb * jax.nn.silu(gcb)], axis=-1) @ w_out
    return y, yc


def axial_rope_tables(rows):
    row = jnp.repeat(jnp.arange(rows, dtype=jnp.float32), GRID_W)
    col = jnp.tile(jnp.arange(GRID_W, dtype=jnp.float32), rows)
    inv = ROPE_THETA ** (-jnp.arange(ROPE_FREQS, dtype=jnp.float32) / ROPE_FREQS)
    ang = jnp.stack([row[:, None] * inv, col[:, None] * inv], axis=1)
    return jnp.cos(ang), jnp.sin(ang)


def apply_axial_rope(t, cos, sin):
    B_, T, H, _ = t.shape
    t = t.reshape(B_, T, H, 2, 2, ROPE_FREQS)
    t1 = t[..., 0, :]
    t2 = t[..., 1, :]
    cs = cos[None, :, None]
    sn = sin[None, :, None]
    out = jnp.stack([t1 * cs - t2 * sn, t2 * cs + t1 * sn], axis=-2)
    return out.reshape(B_, T, H, RET_QK_DIM)


def retention_chunks(q, k, v, log_g, s0, reverse):
    B_, T, H, dk = q.shape
    dv = v.shape[-1]
    n = T // CHUNK

    def to_chunks(t):
        return t.reshape(B_, n, CHUNK, H, t.shape[-1]).transpose(1, 0, 3, 2, 4)

    lg = log_g.astype(jnp.float32)
    idx = jnp.arange(CHUNK, dtype=jnp.float32)
    diff = idx[None, :] - idx[:, None]
    if reverse:
        mask = diff > 0
        expo = diff
        q_exp = CHUNK - idx
        k_exp = idx
    else:
        mask = diff <= 0
        expo = -diff
        q_exp = idx + 1.0
        k_exp = (CHUNK - 1.0) - idx
    dmat = jnp.where(mask, jnp.exp(lg[:, None, None] * jnp.where(mask, expo, 0.0)), 0.0)
    q_dec = jnp.exp(lg[:, None] * q_exp)[:, :, None]
    k_dec = jnp.exp(lg[:, None] * k_exp)[:, :, None]
    c_dec = jnp.exp(lg * CHUNK)[:, None, None]

    def step(s, qkv):
        qn, kn, vn = qkv
        sc = jnp.einsum('bhid,bhjd->bhij', qn, kn) * dmat
        o = jnp.einsum('bhij,bhjv->bhiv', sc, vn) + jnp.einsum('bhid,bhdv->bhiv', qn * q_dec, s)
        s_new = c_dec * s + jnp.einsum('bhjd,bhjv->bhdv', kn * k_dec, vn)
        return s_new, o

    if s0 is None:
        s0 = jnp.zeros((B_, H, dk, dv), jnp.float32)
    s_end, o = lax.scan(step, s0, (to_chunks(q), to_chunks(k), to_chunks(v)), reverse=reverse)
    o = o.transpose(1, 0, 3, 2, 4).reshape(B_, T, H, dv)
    return o, s_end


def retention_state(k, v, log_g, reverse):
    T = k.shape[1]
    pos = jnp.arange(T, dtype=jnp.float32)
    expo = pos if reverse else (T - 1.0) - pos
    w = jnp.exp(log_g.astype(jnp.float32)[:, None] * expo[None, :])
    return jnp.einsum('bthd,ht,bthv->bhdv', k, w, v)


def head_norm(o):
    mu = jnp.mean(o, axis=-1, keepdims=True)
    var = jnp.mean(jnp.square(o - mu), axis=-1, keepdims=True)
    return (o - mu) * lax.rsqrt(var + EPS)


def retention_mixer(h, hc, w_in, w_out, log_gamma, cos, sin, need_ctx):
    def proj(z):
        B_, T, _ = z.shape
        u = (z @ w_in).astype(jnp.float32)
        q, k, v, g = jnp.split(u, [RET_QK, 2 * RET_QK, 2 * RET_QK + RET_MIX], axis=-1)
        q = q.reshape(B_, T, RET_HEADS, RET_QK_DIM)
        k = k.reshape(B_, T, RET_HEADS, RET_QK_DIM) * (RET_QK_DIM ** -0.5)
        v = v.reshape(B_, T, RET_HEADS, RET_V_DIM)
        return q, k, v, g

    q, k, v, g = proj(h)
    q = apply_axial_rope(q, cos, sin)
    k = apply_axial_rope(k, cos, sin)
    qc, kc, vc, gc = proj(hc)
    o = None
    oc = None
    for d in range(2):
        rev = d == 1
        lg = log_gamma[d]
        if need_ctx:
            ocd, s_ctx = retention_chunks(qc, kc, vc, lg, None, rev)
            oc = ocd if oc is None else oc + ocd
        else:
            s_ctx = retention_state(kc, vc, lg, rev)
        od, _ = retention_chunks(q, k, v, lg, s_ctx, rev)
        o = od if o is None else o + od
    B_, T = h.shape[0], h.shape[1]
    y = (head_norm(o).reshape(B_, T, RET_MIX) * jax.nn.silu(g)).astype(h.dtype) @ w_out
    if not need_ctx:
        return y, None
    Tc = hc.shape[1]
    yc = (head_norm(oc).reshape(B_, Tc, RET_MIX) * jax.nn.silu(gc)).astype(hc.dtype) @ w_out
    return y, yc


def setup_inputs(seed: int = 0):
    key = jax.random.key(seed)
    ks = jax.random.split(key, 21)
    n_even = (DEPTH + 1) // 2
    n_odd = DEPTH // 2
    f32 = jnp.float32

    def nrm(k, shape, scale):
        return scale * jax.random.normal(k, shape, f32)

    u = jax.random.uniform(ks[16], (n_even, 2, LRU_WIDTH), f32, 0.9, 0.999)
    a = u ** (1.0 / LRU_C)
    base_lg = jnp.log1p(-jnp.exp2(-5.0 - jnp.arange(RET_HEADS, dtype=f32)))
    return {
        'x': nrm(ks[0], (BATCH, SEQ, D_MODEL), 1.0),
        'c': nrm(ks[1], (BATCH, D_MODEL), 1.0),
        'ctx': nrm(ks[2], (BATCH, CTX_LEN, D_MODEL), 1.0),
        'c_ctx': nrm(ks[3], (D_MODEL,), 1.0),
        'mod_w': nrm(ks[4], (DEPTH, D_MODEL, 3 * D_MODEL), D_MODEL ** -0.5),
        'mod_b': nrm(ks[5], (DEPTH, 3 * D_MODEL), 0.01),
        'pre_g': 1.0 + nrm(ks[6], (DEPTH, D_MODEL), 0.02),
        'post_g': 1.0 + nrm(ks[7], (DEPTH, D_MODEL), 0.02),
        'mix_w_in': nrm(ks[8], (n_even, D_MODEL, EVEN_IN), D_MODEL ** -0.5),
        'mix_w_out': nrm(ks[9], (n_even, EVEN_MIX, D_MODEL), EVEN_MIX ** -0.5),
        'conv_w': nrm(ks[10], (n_even, CONV_W, LRU_WIDTH), CONV_W ** -0.5),
        'conv_b': nrm(ks[11], (n_even, LRU_WIDTH), 0.01),
        'lru_wa': nrm(ks[12], (n_even, 2, LRU_HEADS, LRU_HEAD_DIM, LRU_HEAD_DIM), LRU_HEAD_DIM ** -0.5),
        'lru_ba': nrm(ks[13], (n_even, 2, LRU_WIDTH), 0.01),
        'lru_wx': nrm(ks[14], (n_even, 2, LRU_HEADS, LRU_HEAD_DIM, LRU_HEAD_DIM), LRU_HEAD_DIM ** -0.5),
        'lru_bx': nrm(ks[15], (n_even, 2, LRU_WIDTH), 0.01),
        'lru_lam': jnp.log(a) - jnp.log1p(-a),
        'fnet_w': nrm(ks[17], (n_even, FNET_GROUPS, FNET_GROUP_DIM, FNET_GROUP_DIM), FNET_GROUP_DIM ** -0.5),
        'ret_w_in': nrm(ks[18], (n_odd, D_MODEL, ODD_IN), D_MODEL ** -0.5),
        'ret_w_out': nrm(ks[19], (n_odd, RET_MIX, D_MODEL), RET_MIX ** -0.5),
        'ret_log_gamma': base_lg * jnp.exp(nrm(ks[20], (n_odd, 2, RET_HEADS), 0.05)),
    }


def reference(x, c, ctx, c_ctx, mod_w, mod_b, pre_g, post_g, mix_w_in, mix_w_out, conv_w, conv_b,
              lru_wa, lru_ba, lru_wx, lru_bx, lru_lam, fnet_w, ret_w_in, ret_w_out, ret_log_gamma):
    ROWS = x.shape[1] // GRID_W
    cos, sin = axial_rope_tables(ROWS)
    xc = ctx
    for layer in range(DEPTH):
        need_ctx = layer < DEPTH - 1
        shift, scale, gate = ada_mod(c, mod_w[layer], mod_b[layer])
        shift_c, scale_c, gate_c = ada_mod(c_ctx, mod_w[layer], mod_b[layer])
        h = rms_norm(x, pre_g[layer]) * (1.0 + scale[:, None]) + shift[:, None]
        hc = rms_norm(xc, pre_g[layer]) * (1.0 + scale_c) + shift_c
        if layer % 2 == 0:
            e = layer // 2
            y, yc = even_mixer(h, hc, mix_w_in[e], mix_w_out[e], conv_w[e], conv_b[e],
                               lru_wa[e], lru_ba[e], lru_wx[e], lru_bx[e], lru_lam[e], fnet_w[e], need_ctx)
        else:
            j = layer // 2
            y, yc = retention_mixer(h, hc, ret_w_in[j], ret_w_out[j], ret_log_gamma[j], cos, sin, need_ctx)
        x = x + gate[:, None] * rms_norm(y, post_g[layer])
        if need_ctx:
            xc = xc + gate_c * rms_norm(yc, post_g[layer])
    return x
```

```python
import contextlib
import numpy as np
import ml_dtypes
import concourse.bass as bass
import concourse.mybir as mybir
from concourse.bass_utils import run_bass_kernel_spmd

F32 = mybir.dt.float32
BF16 = mybir.dt.bfloat16
ALU = mybir.AluOpType
AF = mybir.ActivationFunctionType
AX = mybir.AxisListType
NPBF = ml_dtypes.bfloat16

D = 1024
EPS = 1e-6
ENGS = ("pe", "act", "dve", "pool", "sp")


class _Op:
    __slots__ = ("eng", "fn", "deps", "is_dma", "chan", "signal", "val", "idx")

    def __init__(self, eng, fn, is_dma, chan):
        self.eng = eng
        self.fn = fn
        self.deps = []
        self.is_dma = is_dma
        self.chan = chan
        self.signal = False
        self.val = None


class Prog:
    def __init__(self, nc):
        self.nc = nc
        self.stack = contextlib.ExitStack()
        self.ops = {e: [] for e in ENGS}
        self.last_w = {}
        self.readers = {}
        self.all_ops = []
        self.n_tiles = 0

    def sb(self, shape, dtype=F32, name=None):
        self.n_tiles += 1
        return self.stack.enter_context(self.nc.sbuf_tensor(name or f"auto_sb{self.n_tiles}", list(shape), dtype))

    def ps(self, shape, dtype=F32, name=None):
        self.n_tiles += 1
        return self.stack.enter_context(self.nc.psum_tensor(name or f"auto_ps{self.n_tiles}", list(shape), dtype))

    def op(self, eng, fn, reads=(), writes=(), dma=False, chan=None):
        o = _Op(eng, fn, dma, chan)
        deps = []
        for r in reads:
            w = self.last_w.get(r)
            if w is not None:
                deps.append(w)
        for w_ in writes:
            w = self.last_w.get(w_)
            if w is not None:
                deps.append(w)
            deps.extend(self.readers.get(w_, ()))
        seen = set()
        for d in deps:
            if id(d) in seen:
                continue
            seen.add(id(d))
            o.deps.append(d)
        for r in reads:
            self.readers.setdefault(r, []).append(o)
        for w_ in writes:
            self.last_w[w_] = o
            self.readers[w_] = []
        self.ops[eng].append(o)
        self.all_ops.append(o)
        return o

    def dma(self, eng, out, in_, reads=(), writes=(), chan=None, **kw):
        assert chan is not None
        return self.op(eng, lambda e: e.dma_start(out=out, in_=in_, **kw), reads, writes, dma=True, chan=chan)

    def emit(self):
        nc = self.nc
        for o in self.all_ops:
            for d in o.deps:
                d.signal = True
            if o.is_dma:
                o.signal = True
        sems = {}
        counts = {}
        for o in self.all_ops:
            if not o.signal:
                continue
            key = ("d_" + str(o.chan)) if o.is_dma else ("e_" + o.eng)
            counts[key] = counts.get(key, 0) + (16 if o.is_dma else 1)
            o.val = (key, counts[key])
        for k in counts:
            sems[k] = self.stack.enter_context(nc.semaphore("s_" + k))
        final = dict(counts)
        self.n_sems = len(sems)
        block = self.stack.enter_context(nc.Block())

        def make(engname):
            def body(e):
                known = {}
                for o in self.ops[engname]:
                    for d in o.deps:
                        key, v = d.val
                        if known.get(key, 0) >= v:
                            continue
                        known[key] = v
                        e.wait_ge(sems[key], v)
                    ins = o.fn(e)
                    if o.signal:
                        ins.then_inc(sems[o.val[0]], 16 if o.is_dma else 1)
                if engname == "sp":
                    for key, v in final.items():
                        if key.startswith("d_") and known.get(key, 0) < v:
                            e.wait_ge(sems[key], v)
            return body

        block.sync(make("sp"))
        if self.ops["pe"]:
            block.tensor(make("pe"))
        if self.ops["act"]:
            block.scalar(make("act"))
        if self.ops["dve"]:
            block.vector(make("dve"))
        if self.ops["pool"]:
            block.gpsimd(make("pool"))
        self.stack.close()


def build_T(NT, n_ctx_tiles, has_tail, has_head):
    nc = bass.Bass("TRN2", target_bir_lowering=False)
    x_d = nc.dram_tensor("x", [NT * 128, D], F32, kind="ExternalInput").ap()
    cv_d = nc.dram_tensor("cv", [128, 16], F32, kind="ExternalInput").ap()
    if has_tail:
        mT_d = nc.dram_tensor("mT", [NT, 128, 16, 128], BF16, kind="ExternalInput").ap()
        wout_d = nc.dram_tensor("w_out", [2 * D, D], F32, kind="ExternalInput").ap()
        modw_p_d = nc.dram_tensor("modw_p", [D, D], F32, kind="ExternalInput").ap()
        modb_p_d = nc.dram_tensor("modb_p", [128, D], F32, kind="ExternalInput").ap()
        postg_d = nc.dram_tensor("postg", [128, D], F32, kind="ExternalInput").ap()
        xo_d = nc.dram_tensor("xo", [NT * 128, D], F32, kind="ExternalOutput").ap()
    if has_head:
        modw_c_d = nc.dram_tensor("modw_c", [D, 2 * D], F32, kind="ExternalInput").ap()
        modb_c_d = nc.dram_tensor("modb_c", [128, 2 * D], F32, kind="ExternalInput").ap()
        preg_d = nc.dram_tensor("preg", [128, D], F32, kind="ExternalInput").ap()
        h_d = nc.dram_tensor("h", [NT * 128, D], BF16, kind="ExternalOutput").ap()

    P = Prog(nc)
    cv = P.sb([128, 16]); sc = P.sb([128, 16])
    ones = P.sb([128, 128]); L = P.sb([128, 16, 128])
    P.dma("sp", cv[:], cv_d[:, :], writes=["cv"], chan="cv")
    P.op("act", lambda e: e.activation(out=sc[:], in_=cv[:], func=AF.Silu), reads=["cv"], writes=["sc"])
    P.op("dve", lambda e: e.memset(ones[:], 1.0), writes=["ones"])
    epsb = P.sb([128, 1], name="epsb")
    P.op("dve", lambda e: e.memset(epsb[:], EPS), writes=["epsb"])
    for i in range(16):
        P.op("dve", lambda e, i=i: e.tensor_scalar(out=L[:, i, :], in0=ones[:], scalar1=sc[:, i:i + 1], scalar2=None, op0=ALU.mult),
             reads=["ones", "sc"], writes=[("L", i)])

    wc = [P.sb([128, 8, 512], name=f"wc{i}") for i in range(2)]
    pm = [P.ps([128, 512], name=f"pm{i}") for i in range(2)]
    n_mod = 0

    def modulation(modw_d, modb_d, ncols, name):
        nonlocal n_mod
        res = P.sb([128, 2, ncols], name=f"sbmod_{name}")
        bias = P.sb([128, ncols], name=f"sbbias_{name}")
        P.dma("sp", bias[:], modb_d[:, :], writes=[f"bias_{name}"], chan=f"b_{name}")
        wv = modw_d.rearrange("(k p) n -> p k n", p=128)
        for n in range(ncols // 512):
            wb = wc[n_mod % 2]; wk = ("wc", n_mod % 2)
            P.dma("sp", wb[:], wv[:, :, n * 512:(n + 1) * 512], writes=[wk], chan=f"wc{n_mod % 2}")
            for s in range(2):
                pb = pm[(2 * n_mod + s) % 2]; pk = ("pm", (2 * n_mod + s) % 2)
                for k in range(8):
                    P.op("pe", lambda e, s=s, k=k, pb=pb, wb=wb: e.matmul(pb[:], lhsT=L[:, s * 8 + k, :], rhs=wb[:, k, :], start=(k == 0), stop=(k == 7)),
                         reads=[("L", s * 8 + k), wk], writes=[pk])
                P.op("dve", lambda e, s=s, n=n, pb=pb: e.tensor_tensor(out=res[:, s, n * 512:(n + 1) * 512], in0=pb[:], in1=bias[:, n * 512:(n + 1) * 512], op=ALU.add),
                     reads=[pk, f"bias_{name}"], writes=[f"mod_{name}"])
            n_mod += 1
        return res

    if has_tail:
        gate = modulation(modw_p_d, modb_p_d, D, "p")
        postg = P.sb([128, D]); gp = P.sb([128, 2, D])
        P.dma("sp", postg[:], postg_d[:, :], writes=["postg"], chan="postg")
        for s in range(2):
            P.op("dve", lambda e, s=s: e.tensor_tensor(out=gp[:, s, :], in0=gate[:, s, :], in1=postg[:], op=ALU.mult),
                 reads=["mod_p", "postg"], writes=["gp"])
        wo = P.sb([128, 16, D], BF16); wst = [P.sb([128, D], name=f"wst{i}") for i in range(2)]
        wov = wout_d.rearrange("(k p) n -> p k n", p=128)
        for k in range(16):
            P.dma("pool", wst[k % 2][:], wov[:, k, :], writes=[("wst", k % 2)], chan=f"wst{k % 2}")
            P.op("act", lambda e, k=k: e.activation(out=wo[:, k, :], in_=wst[k % 2][:], func=AF.Copy), reads=[("wst", k % 2)], writes=["wo"])
    if has_head:
        ss_mod = modulation(modw_c_d, modb_c_d, 2 * D, "c")
        preg = P.sb([128, D]); A = P.sb([128, 2, D])
        P.dma("sp", preg[:], preg_d[:, :], writes=["preg"], chan="preg")
        for s in range(2):
            P.op("dve", lambda e, s=s: e.scalar_tensor_tensor(out=A[:, s, :], in0=ss_mod[:, s, D:2 * D], scalar=1.0, in1=preg[:], op0=ALU.add, op1=ALU.mult),
                 reads=["mod_c", "preg"], writes=["A"])

    NB = 2
    xin = [P.sb([128, D], name=f"xin{i}") for i in range(NB)]
    junk = [P.sb([128, D], name=f"junk{i}") for i in range(NB)]
    st = [P.sb([128, 4], name=f"st{i}") for i in range(NB)]
    if has_tail:
        mt = [P.sb([128, 16, 128], BF16, name=f"mt{i}") for i in range(NB)]
        py = [P.ps([128, D], name=f"py{i}") for i in range(NB)]
        tt = [P.sb([128, D], name=f"tt{i}") for i in range(NB)]
        xn = [P.sb([128, D], name=f"xn{i}") for i in range(NB)]
    if has_head:
        hb = [P.sb([128, D], BF16, name=f"hb{i}") for i in range(NB)]
        t2 = [P.sb([128, D], name=f"t2{i}") for i in range(NB)]

    for i in range(NT):
        b = i % NB
        s = 1 if i >= NT - n_ctx_tiles else 0
        rows = slice(i * 128, (i + 1) * 128)
        P.dma("sp", xin[b][:], x_d[rows, :], writes=[("xin", b)], chan=f"xin{b}")
        cur = xin[b]; curk = ("xin", b)
        if has_tail:
            P.dma("sp", mt[b][:], mT_d[i, :, :, :], writes=[("mt", b)], chan=f"mt{b}")
            for n in range(2):
                for k in range(16):
                    P.op("pe", lambda e, b=b, n=n, k=k: e.matmul(py[b][:, n * 512:(n + 1) * 512], lhsT=mt[b][:, k, :], rhs=wo[:, k, n * 512:(n + 1) * 512], start=(k == 0), stop=(k == 15)),
                         reads=[("mt", b), "wo"], writes=[("py", b, n)])
            P.op("act", lambda e, b=b: e.activation(out=junk[b][:], in_=py[b][:], func=AF.Square), reads=[("py", b, 0), ("py", b, 1)], writes=[("junk", b)])
            P.op("dve", lambda e, b=b: e.reduce_sum(out=st[b][:, 0:1], in_=junk[b][:], axis=AX.X), reads=[("junk", b)], writes=[("st", b, 0)])
            P.op("act", lambda e, b=b: e.activation(out=st[b][:, 1:2], in_=st[b][:, 0:1], func=AF.Sqrt, bias=epsb[:, 0:1], scale=1.0 / D), reads=[("st", b, 0), "epsb"], writes=[("st", b, 1)])
            P.op("dve", lambda e, b=b: e.reciprocal(out=st[b][:, 0:1], in_=st[b][:, 1:2]), reads=[("st", b, 1)], writes=[("st", b, 0)])
            P.op("dve", lambda e, b=b, s=s: e.scalar_tensor_tensor(out=tt[b][:], in0=py[b][:], scalar=st[b][:, 0:1], in1=gp[:, s, :], op0=ALU.mult, op1=ALU.mult),
                 reads=[("py", b, 0), ("py", b, 1), ("st", b, 0), "gp"], writes=[("tt", b)])
            P.op("dve", lambda e, b=b: e.tensor_tensor(out=xn[b][:], in0=tt[b][:], in1=xin[b][:], op=ALU.add), reads=[("tt", b), ("xin", b)], writes=[("xn", b)])
            P.dma("pool", xo_d[rows, :], xn[b][:], reads=[("xn", b)], chan=f"xo{b}")
            cur = xn[b]; curk = ("xn", b)
        if has_head:
            P.op("act", lambda e, b=b, cur=cur: e.activation(out=junk[b][:], in_=cur[:], func=AF.Square), reads=[curk], writes=[("junk", b)])
            P.op("dve", lambda e, b=b: e.reduce_sum(out=st[b][:, 2:3], in_=junk[b][:], axis=AX.X), reads=[("junk", b)], writes=[("st", b, 2)])
            P.op("act", lambda e, b=b: e.activation(out=st[b][:, 3:4], in_=st[b][:, 2:3], func=AF.Sqrt, bias=epsb[:, 0:1], scale=1.0 / D), reads=[("st", b, 2), "epsb"], writes=[("st", b, 3)])
            P.op("dve", lambda e, b=b: e.reciprocal(out=st[b][:, 2:3], in_=st[b][:, 3:4]), reads=[("st", b, 3)], writes=[("st", b, 2)])
            P.op("dve", lambda e, b=b, s=s, cur=cur: e.scalar_tensor_tensor(out=t2[b][:], in0=cur[:], scalar=st[b][:, 2:3], in1=A[:, s, :], op0=ALU.mult, op1=ALU.mult),
                 reads=[curk, ("st", b, 2), "A"], writes=[("t2", b)])
            P.op("dve", lambda e, b=b, s=s: e.tensor_tensor(out=hb[b][:], in0=t2[b][:], in1=ss_mod[:, s, 0:D], op=ALU.add), reads=[("t2", b), "mod_c"], writes=[("hb", b)])
            P.dma("pool", h_d[rows, :], hb[b][:], reads=[("hb", b)], chan=f"h{b}")
    P.emit()
    return nc


QK, DV = 256, 512


def build_Modd(n_lat, need_ctx, stop_after=None):
    NCH = 2 + n_lat
    NTOK = NCH * 128
    nc = bass.Bass("TRN2", target_bir_lowering=False)
    di = lambda n, sh, dt=F32: nc.dram_tensor(n, sh, dt, kind="ExternalInput").ap()
    hT_d = di("hT", [NCH, 128, 8, 128], BF16)
    w_d = {n: di(n, [D, QK]) for n in ("wq", "wqp", "wk", "wkp")}
    wv_d = di("wv", [D, DV]); wg_d = di("wg", [D, DV])
    cos_d = di("cosT", [NCH, 128, 2, 128]); sin_d = di("sinT", [NCH, 128, 2, 128])
    lg_d = di("lg", [128, 2])
    expo_d = di("expo", [128, 2, 128]); mask_d = di("mask", [128, 2, 128])
    vexp_d = di("vexp", [128, 4])
    ident_d = di("ident", [128, 128], BF16)
    m_d = nc.dram_tensor("m", [NTOK, DV], BF16, kind="ExternalOutput").ap()
    of_d = nc.dram_tensor("ofwd", [NTOK, DV], F32, kind="ExternalOutput").ap()

    P = Prog(nc)
    wst = [P.sb([128, 8, 512], name=f"mo_wst{i}") for i in range(2)]
    wb = {}
    nst = 0
    for n, ap_, width in [("wq", w_d["wq"], QK), ("wqp", w_d["wqp"], QK), ("wk", w_d["wk"], QK), ("wkp", w_d["wkp"], QK), ("wv", wv_d, DV), ("wg", wg_d, DV)]:
        t = P.sb([128, 8, width], BF16, name=f"mo_{n}b")
        st_ = wst[nst % 2]; sk = ("mo_wst", nst % 2)
        P.dma("sp", st_[:, :, 0:width], ap_.rearrange("(k p) n -> p k n", p=128), writes=[sk], chan=f"mo_wst{nst % 2}")
        P.op("act", lambda e, t=t, st_=st_, width=width: e.activation(out=t[:], in_=st_[:, :, 0:width], func=AF.Copy), reads=[sk], writes=[("w", n)])
        wb[n] = t; nst += 1
    lg = P.sb([128, 2], name="mo_lg"); expo = P.sb([128, 2, 128], name="mo_expo"); mask = P.sb([128, 2, 128], name="mo_mask")
    vexp = P.sb([128, 4], name="mo_vexp"); ident = P.sb([128, 128], BF16, name="mo_ident")
    P.dma("sp", lg[:], lg_d[:, :], writes=["lg"], chan="mo_c0")
    P.dma("sp", expo[:], expo_d[:, :, :], writes=["expo"], chan="mo_c1")
    P.dma("sp", mask[:], mask_d[:, :, :], writes=["mask"], chan="mo_c2")
    P.dma("sp", vexp[:], vexp_d[:, :], writes=["vexp"], chan="mo_c3")
    P.dma("sp", ident[:], ident_d[:, :], writes=["ident"], chan="mo_c4")
    dmat = P.sb([128, 2, 128], name="mo_dmat"); dtmp = P.sb([128, 2, 128], name="mo_dtmp")
    vdec = P.sb([128, 4], name="mo_vdec"); cdec = P.sb([128, 2], name="mo_cdec")
    for dr in range(2):
        P.op("act", lambda e, dr=dr: e.activation(out=dtmp[:, dr, :], in_=expo[:, dr, :], func=AF.Exp, scale=lg[:, dr:dr + 1]), reads=["expo", "lg"], writes=[("dtmp", dr)])
        P.op("dve", lambda e, dr=dr: e.tensor_tensor(out=dmat[:, dr, :], in0=dtmp[:, dr, :], in1=mask[:, dr, :], op=ALU.mult), reads=[("dtmp", dr), "mask"], writes=[("dmat", dr)])
        for qk in range(2):
            col = qk * 2 + dr
            P.op("act", lambda e, col=col, dr=dr: e.activation(out=vdec[:, col:col + 1], in_=vexp[:, col:col + 1], func=AF.Exp, scale=lg[:, dr:dr + 1]), reads=["vexp", "lg"], writes=[("vdec", col)])
        P.op("act", lambda e, dr=dr: e.activation(out=cdec[:, dr:dr + 1], in_=lg[:, dr:dr + 1], func=AF.Exp, scale=128.0), reads=["lg"], writes=[("cdec", dr)])
    epsb = P.sb([128, 1], name="mo_eps")
    P.op("dve", lambda e: e.memset(epsb[:], EPS), writes=["epsb"])
    ones5 = P.sb([128, DV], name="mo_ones")
    P.op("dve", lambda e: e.memset(ones5[:], 1.0), writes=["ones5"])
    if stop_after == "const":
        P.dma("pool", of_d[0:128, 0:128], dmat[:, 0, :], reads=[("dmat", 0)], chan="dbg0")
        P.dma("pool", of_d[0:128, 128:256], dmat[:, 1, :], reads=[("dmat", 1)], chan="dbg1")
        P.dma("pool", of_d[128:256, 0:4], vdec[:, :], reads=[("vdec", i) for i in range(4)], chan="dbg2")
        P.dma("pool", of_d[128:256, 4:6], cdec[:, :], reads=[("cdec", 0), ("cdec", 1)], chan="dbg3")
        P.emit()
        return nc

    hT = [P.sb([128, 8, 128], BF16, name=f"mo_hT{i}") for i in range(2)]
    cs = [P.sb([128, 2, 128], name=f"mo_cos{i}") for i in range(2)]
    sn = [P.sb([128, 2, 128], name=f"mo_sin{i}") for i in range(2)]
    pq = P.ps([128, 512], name="mo_pqk"); pk = pq
    psk = P.ps([128, 512], name="mo_psk")
    pv = P.ps([128, 512], name="mo_pvg"); pg = pv
    poa = P.ps([128, 512], name="mo_poa"); poe = P.ps([128, 512], name="mo_poe"); pst = P.ps([128, 512], name="mo_pst")
    t1 = P.sb([128, 128], name="mo_t1"); t2 = P.sb([128, 128], name="mo_t2"); t3 = P.sb([128, 128], name="mo_t3")
    qT = P.sb([128, 2, 128], BF16, name="mo_qT"); kT = P.sb([128, 2, 128], BF16, name="mo_kT")
    scd = P.sb([128, 128], BF16, name="mo_scd"); kdk = P.sb([128, QK], BF16, name="mo_kdk")
    vb = P.sb([128, DV], BF16, name="mo_vb"); oi = P.sb([128, DV], name="mo_oi"); o = P.sb([128, DV], name="mo_o")
    S = P.sb([128, 2, DV], name="mo_S"); Sb = P.sb([128, 2, DV], BF16, name="mo_Sb")
    ofl = [P.sb([128, DV], name=f"mo_ofl{i}") for i in range(2)]
    osum = P.sb([128, DV], name="mo_osum"); cen = P.sb([128, DV], name="mo_cen"); junk = P.sb([128, DV], name="mo_junk")
    sg = P.sb([128, DV], name="mo_sg"); mo = [P.sb([128, DV], BF16, name=f"mo_mo{i}") for i in range(2)]
    stt = P.sb([128, 4], name="mo_stt")
    it = 0

    def chunk(ci, dr, final):
        nonlocal it
        b = it % 2; it += 1
        rows = slice(ci * 128, (ci + 1) * 128)
        P.dma("sp", hT[b][:], hT_d[ci, :, :, :], writes=[("hT", b)], chan=f"mo_hT{b}")
        P.dma("sp", cs[b][:], cos_d[ci, :, :, :], writes=[("cs", b)], chan=f"mo_cs{b}")
        P.dma("sp", sn[b][:], sin_d[ci, :, :, :], writes=[("sn", b)], chan=f"mo_sn{b}")
        for (pp, ppk, wa, wp_, dst, dk_, scl) in ((pq, "pqk", "wq", "wqp", qT, "qT", 1.0), (pk, "pqk", "wk", "wkp", kT, "kT", QK ** -0.5)):
            for a in range(2):
                for v_, wn in enumerate((wa, wp_)):
                    cb = (a * 2 + v_) * 128
                    for k in range(8):
                        P.op("pe", lambda e, pp=pp, cb=cb, wn=wn, a=a, k=k, b=b: e.matmul(pp[:, cb:cb + 128], lhsT=wb[wn][:, k, a * 128:(a + 1) * 128], rhs=hT[b][:, k, :], start=(k == 0), stop=(k == 7)),
                             reads=[("w", wn), ("hT", b)], writes=[ppk])
            for a in range(2):
                ca = a * 256
                P.op("dve", lambda e, pp=pp, ca=ca, a=a, b=b: e.tensor_tensor(out=t1[:], in0=pp[:, ca:ca + 128], in1=cs[b][:, a, :], op=ALU.mult),
                     reads=[ppk, ("cs", b)], writes=["t1"])
                P.op("dve", lambda e, pp=pp, ca=ca, a=a, b=b: e.tensor_tensor(out=t2[:], in0=pp[:, ca + 128:ca + 256], in1=sn[b][:, a, :], op=ALU.mult),
                     reads=[ppk, ("sn", b)], writes=["t2"])
                P.op("dve", lambda e: e.tensor_tensor(out=t3[:], in0=t1[:], in1=t2[:], op=ALU.add), reads=["t1", "t2"], writes=["t3"])
                P.op("act", lambda e, dst=dst, a=a, scl=scl: e.activation(out=dst[:, a, :], in_=t3[:], func=AF.Copy, scale=scl), reads=["t3"], writes=[(dk_, a)])
        for k in range(8):
            P.op("pe", lambda e, k=k, b=b: e.matmul(pv[:], lhsT=hT[b][:, k, :], rhs=wb["wv"][:, k, :], start=(k == 0), stop=(k == 7)), reads=[("hT", b), ("w", "wv")], writes=["pvg"])
        P.op("act", lambda e: e.activation(out=vb[:], in_=pv[:], func=AF.Copy), reads=["pvg"], writes=["vb"])
        if stop_after == "proj":
            for a in range(2):
                P.op("dve", lambda e, a=a: e.tensor_copy(out=o[:, a * 128:(a + 1) * 128], in_=qT[:, a, :]), reads=[("qT", a)], writes=[("odbg", a)])
                P.op("dve", lambda e, a=a: e.tensor_copy(out=o[:, 256 + a * 128:384 + a * 128], in_=kT[:, a, :]), reads=[("kT", a)], writes=[("odbg", 2 + a)])
            P.dma("pool", of_d[rows, :], o[:], reads=[("odbg", i) for i in range(4)], writes=[("of", ci)], chan="mo_of")
            return
        for a in range(2):
            P.op("pe", lambda e, a=a: e.matmul(psk[:, 0:128], lhsT=kT[:, a, :], rhs=qT[:, a, :], start=(a == 0), stop=(a == 1)), reads=[("kT", a), ("qT", a)], writes=["psk"])
        for a in range(2):
            P.op("pe", lambda e, a=a: e.matmul(psk[:, 128 + a * 128:256 + a * 128], lhsT=kT[:, a, :], rhs=ident[:], start=True, stop=True), reads=[("kT", a), "ident"], writes=["psk"])
        P.op("dve", lambda e: e.tensor_tensor(out=scd[:], in0=psk[:, 0:128], in1=dmat[:, dr, :], op=ALU.mult), reads=["psk", ("dmat", dr)], writes=["scd"])
        P.op("dve", lambda e: e.tensor_scalar(out=kdk[:], in0=psk[:, 128:384], scalar1=vdec[:, 2 + dr:3 + dr], scalar2=None, op0=ALU.mult),
             reads=["psk", ("vdec", 2 + dr)], writes=["kdk"])
        P.op("pe", lambda e: e.matmul(poa[:], lhsT=scd[:], rhs=vb[:], start=True, stop=True), reads=["scd", "vb"], writes=["poa"])
        for a in range(2):
            P.op("pe", lambda e, a=a: e.matmul(poe[:], lhsT=qT[:, a, :], rhs=Sb[:, a, :], start=(a == 0), stop=(a == 1)), reads=[("qT", a), ("Sb", a)], writes=["poe"])
        P.op("act", lambda e: e.activation(out=oi[:], in_=poa[:], func=AF.Copy), reads=["poa"], writes=["oi"])
        P.op("dve", lambda e: e.scalar_tensor_tensor(out=o[:], in0=poe[:], scalar=vdec[:, dr:dr + 1], in1=oi[:], op0=ALU.mult, op1=ALU.add),
             reads=["poe", ("vdec", dr), "oi"], writes=["o"])
        for a in range(2):
            P.op("pe", lambda e, a=a: e.matmul(pst[:], lhsT=kdk[:, a * 128:(a + 1) * 128], rhs=vb[:], start=True, stop=True), reads=["kdk", "vb"], writes=["pst"])
            P.op("dve", lambda e, a=a: e.scalar_tensor_tensor(out=S[:, a, :], in0=S[:, a, :], scalar=cdec[:, dr:dr + 1], in1=pst[:], op0=ALU.mult, op1=ALU.add),
                 reads=[("S", a), ("cdec", dr), "pst"], writes=[("S", a)])
            P.op("act", lambda e, a=a: e.activation(out=Sb[:, a, :], in_=S[:, a, :], func=AF.Copy), reads=[("S", a)], writes=[("Sb", a)])
        if not final:
            P.dma("pool", of_d[rows, :], o[:], reads=["o"], writes=[("of", ci)], chan="mo_of")
            return
        if ci < 2 and not need_ctx:
            return
        P.dma("pool", ofl[b][:], of_d[rows, :], reads=[("of", ci)], writes=[("ofl", b)], chan=f"mo_ofl{b}")
        P.op("dve", lambda e, b=b: e.tensor_tensor(out=osum[:], in0=o[:], in1=ofl[b][:], op=ALU.add), reads=["o", ("ofl", b)], writes=["osum"])
        if stop_after == "Rload":
            P.dma("pool", of_d[rows, :], osum[:], reads=["osum", ("ofl", b)], writes=[("of2", ci)], chan="mo_of2")
            return
        P.op("dve", lambda e: e.reduce_sum(out=stt[:, 0:1], in_=osum[:], axis=AX.X), reads=["osum"], writes=[("stt", 0)])
        P.op("act", lambda e: e.activation(out=stt[:, 1:2], in_=stt[:, 0:1], func=AF.Copy, scale=-1.0 / DV), reads=[("stt", 0)], writes=[("stt", 1)])
        P.op("dve", lambda e: e.scalar_tensor_tensor(out=cen[:], in0=ones5[:], scalar=stt[:, 1:2], in1=osum[:], op0=ALU.mult, op1=ALU.add),
             reads=["ones5", ("stt", 1), "osum"], writes=["cen"])
        P.op("act", lambda e: e.activation(out=junk[:], in_=cen[:], func=AF.Square), reads=["cen"], writes=["junk"])
        P.op("dve", lambda e: e.reduce_sum(out=stt[:, 2:3], in_=junk[:], axis=AX.X), reads=["junk"], writes=[("stt", 2)])
        P.op("act", lambda e: e.activation(out=stt[:, 3:4], in_=stt[:, 2:3], func=AF.Sqrt, bias=epsb[:, 0:1], scale=1.0 / DV), reads=[("stt", 2), "epsb"], writes=[("stt", 3)])
        P.op("dve", lambda e: e.reciprocal(out=stt[:, 2:3], in_=stt[:, 3:4]), reads=[("stt", 3)], writes=[("stt", 2)])
        for k in range(8):
            P.op("pe", lambda e, k=k, b=b: e.matmul(pg[:], lhsT=hT[b][:, k, :], rhs=wb["wg"][:, k, :], start=(k == 0), stop=(k == 7)), reads=[("hT", b), ("w", "wg")], writes=["pvg"])
        P.op("act", lambda e: e.activation(out=sg[:], in_=pg[:], func=AF.Silu), reads=["pvg"], writes=["sg"])
        P.op("dve", lambda e, b=b: e.scalar_tensor_tensor(out=mo[b][:], in0=cen[:], scalar=stt[:, 2:3], in1=sg[:], op0=ALU.mult, op1=ALU.mult),
             reads=["cen", ("stt", 2), "sg"], writes=[("mo", b)])
        P.dma("pool", m_d[rows, :], mo[b][:], reads=[("mo", b)], chan=f"mo_m{b}")

    def reset_state():
        for a in range(2):
            P.op("dve", lambda e, a=a: e.memset(S[:, a, :], 0.0), writes=[("S", a)])
            P.op("dve", lambda e, a=a: e.memset(Sb[:, a, :], 0.0), writes=[("Sb", a)])

    reset_state()
    if stop_after == "proj":
        chunk(0, 0, False)
        chunk(2, 0, False)
        P.emit()
        return nc
    for ci in range(NCH):
        chunk(ci, 0, False)
    if stop_after == "F":
        P.emit()
        return nc
    reset_state()
    for ci in (1, 0):
        chunk(ci, 1, True)
    for ci in range(NCH - 1, 1, -1):
        chunk(ci, 1, True)
    if not need_ctx:
        z = P.sb([128, DV], BF16, name="mo_zero")
        P.op("dve", lambda e: e.memset(z[:], 0.0), writes=["z"])
        for ci in range(2):
            P.dma("pool", m_d[ci * 128:(ci + 1) * 128, :], z[:], reads=["z"], chan=f"mo_z{ci}")
    P.emit()
    return nc


LRU_CH = 256


def build_LRU(n_lat_chunks):
    NCH = 1 + n_lat_chunks
    NTOK = NCH * LRU_CH
    W_ = NTOK + 8
    off = lambda c: 2 if c == 0 else 262 + (c - 1) * LRU_CH
    nc = bass.Bass("TRN2", target_bir_lowering=False)
    di = lambda n, sh, dt=F32: nc.dram_tensor(n, sh, dt, kind="ExternalInput").ap()
    hT_d = di("hT", [NCH, 128, 8, LRU_CH], BF16)
    wxa_d = di("wxa", [D, 128]); wga_d = di("wga", [D, 128])
    cw_d = di("convw", [128, 4]); cb_d = di("convb", [128, 1])
    wa_d = di("wa", [128, 2, 128]); wx_d = di("wx", [128, 2, 128])
    ba_d = di("ba", [128, 2]); bx_d = di("bx", [128, 2]); lam_d = di("lam", [128, 2])
    out_d = nc.dram_tensor("mA", [128, NTOK], BF16, kind="ExternalOutput").ap()

    P = Prog(nc)
    wst = P.sb([128, 8, 128], name="l_wst"); wxa = P.sb([128, 8, 128], BF16, name="l_wxa"); wga = P.sb([128, 8, 128], BF16, name="l_wga")
    for nm, src, dst in (("wxa", wxa_d, wxa), ("wga", wga_d, wga)):
        P.dma("sp", wst[:], src.rearrange("(k p) n -> p k n", p=128), writes=["wst"], chan="l_wst")
        P.op("act", lambda e, dst=dst: e.activation(out=dst[:], in_=wst[:], func=AF.Copy), reads=["wst"], writes=[nm])
    gst = P.sb([128, 2, 128], name="l_gst"); wa = P.sb([128, 2, 128], BF16, name="l_wa"); wx = P.sb([128, 2, 128], BF16, name="l_wx")
    for nm, src, dst in (("wa", wa_d, wa), ("wx", wx_d, wx)):
        P.dma("sp", gst[:], src[:, :, :], writes=["gst"], chan="l_gst")
        P.op("act", lambda e, dst=dst: e.activation(out=dst[:], in_=gst[:], func=AF.Copy), reads=["gst"], writes=[nm])
    cw = P.sb([128, 4], name="l_cw"); cb = P.sb([128, 1], name="l_cb")
    ba = P.sb([128, 2], name="l_ba"); bx = P.sb([128, 2], name="l_bx"); lam = P.sb([128, 2], name="l_lam")
    for i, (t, src, nm) in enumerate(((cw, cw_d, "cw"), (cb, cb_d, "cb"), (ba, ba_d, "ba"), (bx, bx_d, "bx"), (lam, lam_d, "lam"))):
        P.dma("sp", t[:], src[:, :], writes=[nm], chan=f"l_p{i}")
    one1 = P.sb([128, 1], name="l_one1"); onesW = P.sb([128, W_], name="l_onesW")
    P.op("dve", lambda e: e.memset(one1[:], 1.0), writes=["one1"])
    P.op("dve", lambda e: e.memset(onesW[:], 1.0), writes=["onesW"])
    sp1 = P.sb([128, 2], name="l_sp1"); sp2 = P.sb([128, 2], name="l_sp2"); sdec = P.sb([128, 2], name="l_sdec")
    P.op("act", lambda e: e.activation(out=sp1[:], in_=lam[:], func=AF.Exp, scale=-1.0), reads=["lam"], writes=["sp1"])
    P.op("act", lambda e: e.activation(out=sp2[:], in_=sp1[:], func=AF.Ln, bias=one1[:, 0:1]), reads=["sp1", "one1"], writes=["sp2"])
    P.op("act", lambda e: e.activation(out=sdec[:], in_=sp2[:], func=AF.Copy, scale=-8.0), reads=["sp2"], writes=["sdec"])

    xa = P.sb([128, W_], name="l_xa"); z = P.sb([128, W_], name="l_z"); zb = P.sb([128, W_], BF16, name="l_zb")
    sga = P.sb([128, NTOK], name="l_sga"); yf = P.sb([128, NTOK], name="l_yf"); yr = P.sb([128, NTOK], name="l_yr")
    P.op("dve", lambda e: e.memset(xa[:], 0.0), writes=["xa"])
    hT = [P.sb([128, 8, LRU_CH], BF16, name=f"l_hT{i}") for i in range(2)]
    px = P.ps([128, 512], name="l_px"); pgp = P.ps([128, 512], name="l_pg")
    for c in range(NCH):
        b = c % 2
        P.dma("sp", hT[b][:], hT_d[c, :, :, :], writes=[("hT", b)], chan=f"l_hT{b}")
        for k in range(8):
            P.op("pe", lambda e, k=k, b=b: e.matmul(px[:, 0:LRU_CH], lhsT=wxa[:, k, :], rhs=hT[b][:, k, :], start=(k == 0), stop=(k == 7)), reads=["wxa", ("hT", b)], writes=["px"])
        P.op("act", lambda e, c=c: e.activation(out=xa[:, off(c):off(c) + LRU_CH], in_=px[:, 0:LRU_CH], func=AF.Copy), reads=["px"], writes=["xa"])
        for k in range(8):
            P.op("pe", lambda e, k=k, b=b: e.matmul(pgp[:, 0:LRU_CH], lhsT=wga[:, k, :], rhs=hT[b][:, k, :], start=(k == 0), stop=(k == 7)), reads=["wga", ("hT", b)], writes=["pg"])
        P.op("act", lambda e, c=c: e.activation(out=sga[:, c * LRU_CH:(c + 1) * LRU_CH], in_=pgp[:, 0:LRU_CH], func=AF.Silu), reads=["pg"], writes=["sga"])
    L_ = W_ - 4
    P.op("dve", lambda e: e.scalar_tensor_tensor(out=z[:, 2:2 + L_], in0=xa[:, 0:L_], scalar=cw[:, 0:1], in1=onesW[:, 0:L_], op0=ALU.mult, op1=ALU.mult), reads=["xa", "cw", "onesW"], writes=["z"])
    for tap in range(1, 4):
        P.op("dve", lambda e, tap=tap: e.scalar_tensor_tensor(out=z[:, 2:2 + L_], in0=xa[:, tap:tap + L_], scalar=cw[:, tap:tap + 1], in1=z[:, 2:2 + L_], op0=ALU.mult, op1=ALU.add), reads=["xa", "cw", "z"], writes=["z"])
    P.op("dve", lambda e: e.scalar_tensor_tensor(out=z[:, 2:2 + L_], in0=onesW[:, 0:L_], scalar=cb[:, 0:1], in1=z[:, 2:2 + L_], op0=ALU.mult, op1=ALU.add), reads=["onesW", "cb", "z"], writes=["z"])
    P.op("act", lambda e: e.activation(out=zb[:, 2:2 + L_], in_=z[:, 2:2 + L_], func=AF.Copy), reads=["z"], writes=["zb"])
    pr = P.ps([128, 512], name="l_pr"); pi_ = P.ps([128, 512], name="l_pi")
    r = P.sb([128, LRU_CH], name="l_r"); ig = P.sb([128, LRU_CH], name="l_i"); a = P.sb([128, LRU_CH], name="l_a"); a2 = P.sb([128, LRU_CH], name="l_a2")
    om = P.sb([128, LRU_CH], name="l_om"); sq = P.sb([128, LRU_CH], name="l_sq"); t1 = P.sb([128, LRU_CH], name="l_t1"); bt = P.sb([128, LRU_CH], name="l_bt")

    def coeffs(c, dr):
        zs = slice(off(c), off(c) + LRU_CH)
        P.op("pe", lambda e: e.matmul(pr[:, 0:LRU_CH], lhsT=wa[:, dr, :], rhs=zb[:, zs], start=True, stop=True), reads=["wa", "zb"], writes=["pr"])
        P.op("pe", lambda e: e.matmul(pi_[:, 0:LRU_CH], lhsT=wx[:, dr, :], rhs=zb[:, zs], start=True, stop=True), reads=["wx", "zb"], writes=["pi"])
        P.op("act", lambda e: e.activation(out=r[:], in_=pr[:, 0:LRU_CH], func=AF.Sigmoid, bias=ba[:, dr:dr + 1]), reads=["pr", "ba"], writes=["r"])
        P.op("act", lambda e: e.activation(out=ig[:], in_=pi_[:, 0:LRU_CH], func=AF.Sigmoid, bias=bx[:, dr:dr + 1]), reads=["pi", "bx"], writes=["ig"])
        P.op("act", lambda e: e.activation(out=a[:], in_=r[:], func=AF.Exp, scale=sdec[:, dr:dr + 1]), reads=["r", "sdec"], writes=["a"])
        P.op("act", lambda e: e.activation(out=a2[:], in_=a[:], func=AF.Square), reads=["a"], writes=["a2"])
        P.op("dve", lambda e: e.scalar_tensor_tensor(out=om[:], in0=a2[:], scalar=-1.0, in1=onesW[:, 0:LRU_CH], op0=ALU.mult, op1=ALU.add), reads=["a2", "onesW"], writes=["om"])
        P.op("act", lambda e: e.activation(out=sq[:], in_=om[:], func=AF.Sqrt), reads=["om"], writes=["sq"])
        P.op("dve", lambda e: e.tensor_tensor(out=t1[:], in0=ig[:], in1=z[:, zs], op=ALU.mult), reads=["ig", "z"], writes=["t1"])
        P.op("dve", lambda e: e.tensor_tensor(out=bt[:], in0=sq[:], in1=t1[:], op=ALU.mult), reads=["sq", "t1"], writes=["bt"])

    prev = None
    for c in range(NCH):
        coeffs(c, 0)
        ys = slice(c * LRU_CH, (c + 1) * LRU_CH)
        init = 0.0 if prev is None else yf[:, prev:prev + 1]
        P.op("dve", lambda e, ys=ys, init=init: e.tensor_tensor_scan(out=yf[:, ys], data0=a[:], data1=bt[:], initial=init, op0=ALU.mult, op1=ALU.add), reads=["a", "bt", "yf"], writes=["yf"])
        prev = (c + 1) * LRU_CH - 1
    prev = None
    for c in [0] + list(range(NCH - 1, 0, -1)):
        coeffs(c, 1)
        lo, hi = c * LRU_CH, (c + 1) * LRU_CH
        init = 0.0 if prev is None else yr[:, prev:prev + 1]
        P.op("dve", lambda e, lo=lo, hi=hi, init=init: e.tensor_tensor_scan(out=yr[:, lo:hi][:, ::-1], data0=a[:, ::-1], data1=bt[:, ::-1], initial=init, op0=ALU.mult, op1=ALU.add),
             reads=["a", "bt", "yr"], writes=["yr"])
        prev = lo
    ysum = P.sb([128, NTOK], name="l_ys"); mo = P.sb([128, NTOK], BF16, name="l_mo")
    P.op("dve", lambda e: e.tensor_tensor(out=ysum[:], in0=yf[:], in1=yr[:], op=ALU.add), reads=["yf", "yr"], writes=["ysum"])
    P.op("dve", lambda e: e.tensor_tensor(out=mo[:], in0=ysum[:], in1=sga[:], op=ALU.mult), reads=["ysum", "sga"], writes=["mo"])
    P.dma("pool", out_d[:, :], mo[:], reads=["mo"], chan="l_out")
    P.emit()
    return nc


FFT_GC = 256


def build_FFT(N2):
    T = 128 * N2
    nc = bass.Bass("TRN2", target_bir_lowering=False)
    di = lambda n, sh, dt=F32: nc.dram_tensor(n, sh, dt, kind="ExternalInput").ap()
    hX_d = di("hX", [N2, 128, 8, 128], BF16)
    hG_d = di("hG", [128, 128, 8, N2], BF16)
    wxb_d = di("wxb", [D, FFT_GC]); wgb_d = di("wgb", [D, FFT_GC]); wf_d = di("wf", [FFT_GC, FFT_GC])
    cc_d = di("cc", [FFT_GC, FFT_GC]); sc_d = di("sc", [FFT_GC, FFT_GC])
    ma_d = di("ma", [N2, 128, 2 * 128], BF16)
    cn_d = di("cn", [N2, N2], BF16); sn_d = di("sn", [N2, N2], BF16)
    y_d = nc.dram_tensor("yB", [N2, 128, FFT_GC], BF16, kind="ExternalOutput").ap()
    P = Prog(nc)
    scale = 1.0 / np.sqrt(T * FFT_GC)
    wst = P.sb([128, 8, FFT_GC], name="f_wst"); wxb = P.sb([128, 8, FFT_GC], BF16, name="f_wxb"); wgb = P.sb([128, 8, FFT_GC], BF16, name="f_wgb")
    for nm, src, dst in (("wxb", wxb_d, wxb), ("wgb", wgb_d, wgb)):
        P.dma("sp", wst[:], src.rearrange("(k p) n -> p k n", p=128), writes=["wst"], chan="f_wst")
        P.op("act", lambda e, dst=dst: e.activation(out=dst[:], in_=wst[:], func=AF.Copy), reads=["wst"], writes=[nm])
    wf = P.sb([128, 2, FFT_GC], name="f_wf"); cc = P.sb([128, 2, FFT_GC], name="f_cc"); sc = P.sb([128, 2, FFT_GC], name="f_sc")
    P.dma("sp", wf[:], wf_d.rearrange("(k p) n -> p k n", p=128), writes=["wf"], chan="f_c0")
    P.dma("sp", cc[:], cc_d.rearrange("(k p) n -> p k n", p=128), writes=["cc"], chan="f_c1")
    P.dma("sp", sc[:], sc_d.rearrange("(k p) n -> p k n", p=128), writes=["sc"], chan="f_c2")
    pG = P.ps([128, 512], name="f_pG")
    R1 = P.sb([128, 2, 512], BF16, name="f_R1"); R2 = P.sb([128, 2, 512], BF16, name="f_R2")
    for ct in range(2):
        for (tab, tk, c0) in ((cc, "cc", 0), (sc, "sc", 256)):
            for m in range(2):
                P.op("pe", lambda e, tab=tab, m=m, ct=ct, c0=c0: e.matmul(pG[:, c0:c0 + 256], lhsT=tab[:, m, ct * 128:(ct + 1) * 128], rhs=wf[:, m, :], start=(m == 0), stop=(m == 1)),
                     reads=[tk, "wf"], writes=["pG"])
        P.op("act", lambda e, ct=ct: e.activation(out=R1[:, ct, 0:256], in_=pG[:, 0:256], func=AF.Copy), reads=["pG"], writes=[("R1", ct)])
        P.op("act", lambda e, ct=ct: e.activation(out=R1[:, ct, 256:512], in_=pG[:, 256:512], func=AF.Copy, scale=-1.0), reads=["pG"], writes=[("R1", ct)])
        P.op("act", lambda e, ct=ct: e.activation(out=R2[:, ct, 0:256], in_=pG[:, 256:512], func=AF.Copy), reads=["pG"], writes=[("R2", ct)])
        P.op("act", lambda e, ct=ct: e.activation(out=R2[:, ct, 256:512], in_=pG[:, 0:256], func=AF.Copy), reads=["pG"], writes=[("R2", ct)])
    cn = P.sb([N2, N2], BF16, name="f_cn"); sn = P.sb([N2, N2], BF16, name="f_sn")
    P.dma("sp", cn[:], cn_d[:, :], writes=["cn"], chan="f_c3"); P.dma("sp", sn[:], sn_d[:, :], writes=["sn"], chan="f_c4")

    Ba = [P.sb([128, 2, N2, 128], BF16, name=f"f_Ba{ct}") for ct in range(2)]
    hX = [P.sb([128, 8, 128], BF16, name=f"f_hX{i}") for i in range(2)]
    ma = [P.sb([128, 256], BF16, name=f"f_ma{i}") for i in range(2)]
    pX = P.ps([128, 512], name="f_pX"); pA = P.ps([128, 512], name="f_pA")
    Xb = P.sb([128, FFT_GC], BF16, name="f_Xb")
    for t2 in range(N2):
        b = t2 % 2
        P.dma("sp", hX[b][:], hX_d[t2, :, :, :], writes=[("hX", b)], chan=f"f_hX{b}")
        P.dma("sp", ma[b][:], ma_d[t2, :, :], writes=[("ma", b)], chan=f"f_ma{b}")
        for k in range(8):
            P.op("pe", lambda e, k=k, b=b: e.matmul(pX[:, 0:FFT_GC], lhsT=hX[b][:, k, :], rhs=wxb[:, k, :], start=(k == 0), stop=(k == 7)), reads=[("hX", b), "wxb"], writes=["pX"])
        P.op("act", lambda e: e.activation(out=Xb[:], in_=pX[:, 0:FFT_GC], func=AF.Copy), reads=["pX"], writes=["Xb"])
        for ct in range(2):
            P.op("pe", lambda e, ct=ct, b=b: e.matmul(pA[:, ct * 256:(ct + 1) * 256], lhsT=Xb[:, ct * 128:(ct + 1) * 128], rhs=ma[b][:], start=True, stop=True), reads=["Xb", ("ma", b)], writes=["pA"])
        for ct in range(2):
            for ri in range(2):
                P.op("act", lambda e, ct=ct, ri=ri, t2=t2: e.activation(out=Ba[ct][:, ri, t2, :], in_=pA[:, ct * 256 + ri * 128:ct * 256 + (ri + 1) * 128], func=AF.Copy),
                     reads=["pA"], writes=[("Ba", ct)])
    hG = [P.sb([128, 8, N2], BF16, name=f"f_hG{i}") for i in range(2)]
    pD = P.ps([128, 512], name="f_pD"); pY = P.ps([128, 512], name="f_pY"); pGt = P.ps([128, 512], name="f_pGt")
    Db = P.sb([N2, 512], BF16, name="f_Db"); sg = P.sb([N2, FFT_GC], name="f_sg")
    yo = [P.sb([N2, FFT_GC], BF16, name=f"f_yo{i}") for i in range(2)]
    for k1 in range(128):
        b = k1 % 2
        for i, (ct, ri, R, rk) in enumerate(((0, 0, R1, "R1"), (1, 0, R1, "R1"), (0, 1, R2, "R2"), (1, 1, R2, "R2"))):
            P.op("pe", lambda e, ct=ct, ri=ri, R=R, i=i, k1=k1: e.matmul(pD[0:N2, :], lhsT=Ba[ct][:, ri, :, k1], rhs=R[:, ct, :], start=(i == 0), stop=(i == 3)),
                 reads=[("Ba", ct), (rk, ct)], writes=["pD"])
        P.op("act", lambda e: e.activation(out=Db[:], in_=pD[0:N2, :], func=AF.Copy), reads=["pD"], writes=["Db"])
        P.op("pe", lambda e: e.matmul(pY[0:N2, 0:FFT_GC], lhsT=cn[:], rhs=Db[:, 0:256], start=True, stop=False), reads=["cn", "Db"], writes=["pY"])
        P.op("pe", lambda e: e.matmul(pY[0:N2, 0:FFT_GC], lhsT=sn[:], rhs=Db[:, 256:512], start=False, stop=True), reads=["sn", "Db"], writes=["pY"])
        P.dma("sp", hG[b][:], hG_d[k1, :, :, :], writes=[("hG", b)], chan=f"f_hG{b}")
        for k in range(8):
            P.op("pe", lambda e, k=k, b=b: e.matmul(pGt[0:N2, 0:FFT_GC], lhsT=hG[b][:, k, :], rhs=wgb[:, k, :], start=(k == 0), stop=(k == 7)), reads=[("hG", b), "wgb"], writes=["pGt"])
        P.op("act", lambda e: e.activation(out=sg[:], in_=pGt[0:N2, 0:FFT_GC], func=AF.Silu), reads=["pGt"], writes=["sg"])
        P.op("dve", lambda e, b=b: e.scalar_tensor_tensor(out=yo[b][:], in0=pY[0:N2, 0:FFT_GC], scalar=float(scale), in1=sg[:], op0=ALU.mult, op1=ALU.mult),
             reads=["pY", "sg"], writes=[("yo", b)])
        P.dma("pool", y_d[:, k1, :], yo[b][:], reads=[("yo", b)], chan=f"f_out{b}")
    P.emit()
    return nc


def fft_host_inputs(h_seq, w_xb, w_gb, w_f):
    T = h_seq.shape[0]; N2 = T // 128
    h = np.asarray(h_seq).astype(NPBF)
    t1 = np.arange(128); t2 = np.arange(N2); k1 = np.arange(128); k2 = np.arange(N2); mc = np.arange(FFT_GC)
    ang = 2 * np.pi * (((N2 * t1[None, :, None] + t2[:, None, None]) * k1[None, None, :]) % T) / T
    angc = 2 * np.pi * ((mc[:, None] * mc[None, :]) % FFT_GC) / FFT_GC
    angn = 2 * np.pi * ((t2[:, None] * k2[None, :]) % N2) / N2
    return {"hX": np.ascontiguousarray(h.reshape(128, N2, 8, 128).transpose(1, 3, 2, 0)),
            "hG": np.ascontiguousarray(h.reshape(N2, 128, 8, 128).transpose(1, 3, 2, 0)),
            "wxb": np.ascontiguousarray(w_xb, np.float32), "wgb": np.ascontiguousarray(w_gb, np.float32),
            "wf": np.ascontiguousarray(w_f, np.float32),
            "cc": np.cos(angc).astype(np.float32), "sc": np.sin(angc).astype(np.float32),
            "ma": np.concatenate([np.cos(ang), -np.sin(ang)], axis=2).astype(np.float32).astype(NPBF),
            "cn": np.cos(angn).astype(np.float32).astype(NPBF), "sn": np.sin(angn).astype(np.float32).astype(NPBF)}


def build_LRU_full(n_lat_chunks, PSZ=2048):
    NCH = 1 + n_lat_chunks
    NTOK = NCH * LRU_CH
    W_ = NTOK + 8
    off = lambda c: 2 if c == 0 else 262 + (c - 1) * LRU_CH
    nc = bass.Bass("TRN2", target_bir_lowering=False)
    di = lambda n, sh, dt=F32: nc.dram_tensor(n, sh, dt, kind="ExternalInput").ap()
    hT_d = di("hT", [NCH, 128, 8, LRU_CH], BF16)
    wxa_d = di("wxa", [D, 128]); wga_d = di("wga", [D, 128])
    cw_d = di("convw", [128, 4]); cb_d = di("convb", [128, 1])
    wa_d = di("wa", [128, 2, 128]); wx_d = di("wx", [128, 2, 128])
    ba_d = di("ba", [128, 2]); bx_d = di("bx", [128, 2]); lam_d = di("lam", [128, 2])
    out_d = nc.dram_tensor("mA", [128, NTOK], BF16, kind="ExternalOutput").ap()
    P = Prog(nc)
    wst = P.sb([128, 8, 128], name="L_wst"); wxa = P.sb([128, 8, 128], BF16, name="L_wxa"); wga = P.sb([128, 8, 128], BF16, name="L_wga")
    for nm, src, dst in (("wxa", wxa_d, wxa), ("wga", wga_d, wga)):
        P.dma("sp", wst[:], src.rearrange("(k p) n -> p k n", p=128), writes=["wst"], chan="L_wst")
        P.op("act", lambda e, dst=dst: e.activation(out=dst[:], in_=wst[:], func=AF.Copy), reads=["wst"], writes=[nm])
    gst = P.sb([128, 2, 128], name="L_gst"); wa = P.sb([128, 2, 128], BF16, name="L_wa"); wx = P.sb([128, 2, 128], BF16, name="L_wx")
    for nm, src, dst in (("wa", wa_d, wa), ("wx", wx_d, wx)):
        P.dma("sp", gst[:], src[:, :, :], writes=["gst"], chan="L_gst")
        P.op("act", lambda e, dst=dst: e.activation(out=dst[:], in_=gst[:], func=AF.Copy), reads=["gst"], writes=[nm])
    cw = P.sb([128, 4], name="L_cw"); cb = P.sb([128, 1], name="L_cb")
    ba = P.sb([128, 2], name="L_ba"); bx = P.sb([128, 2], name="L_bx"); lam = P.sb([128, 2], name="L_lam")
    for i, (t, src, nm) in enumerate(((cw, cw_d, "cw"), (cb, cb_d, "cb"), (ba, ba_d, "ba"), (bx, bx_d, "bx"), (lam, lam_d, "lam"))):
        P.dma("sp", t[:], src[:, :], writes=[nm], chan=f"L_p{i}")
    one1 = P.sb([128, 1], name="L_one1"); ones = P.sb([128, PSZ], name="L_ones")
    P.op("dve", lambda e: e.memset(one1[:], 1.0), writes=["one1"])
    P.op("dve", lambda e: e.memset(ones[:], 1.0), writes=["ones"])
    sp1 = P.sb([128, 2], name="L_sp1"); sp2 = P.sb([128, 2], name="L_sp2"); sdec = P.sb([128, 2], name="L_sdec")
    P.op("act", lambda e: e.activation(out=sp1[:], in_=lam[:], func=AF.Exp, scale=-1.0), reads=["lam"], writes=["sp1"])
    P.op("act", lambda e: e.activation(out=sp2[:], in_=sp1[:], func=AF.Ln, bias=one1[:, 0:1]), reads=["sp1", "one1"], writes=["sp2"])
    P.op("act", lambda e: e.activation(out=sdec[:], in_=sp2[:], func=AF.Copy, scale=-8.0), reads=["sp2"], writes=["sdec"])

    A = P.sb([128, W_], name="L_A"); B = P.sb([128, W_], name="L_B")
    for p0 in range(0, W_, PSZ):
        n = min(PSZ, W_ - p0)
        P.op("dve", lambda e, p0=p0, n=n: e.memset(A[:, p0:p0 + n], 0.0), writes=["A"])
    hT = [P.sb([128, 8, LRU_CH], BF16, name=f"L_hT{i}") for i in range(2)]
    px = P.ps([128, 512], name="L_px"); pgp = P.ps([128, 512], name="L_pg")
    it = [0]

    def load_h(c):
        b = it[0] % 2; it[0] += 1
        P.dma("sp", hT[b][:], hT_d[c, :, :, :], writes=[("hT", b)], chan=f"L_hT{b}")
        return b
    for c in range(NCH):
        b = load_h(c)
        for k in range(8):
            P.op("pe", lambda e, k=k, b=b: e.matmul(px[:, 0:LRU_CH], lhsT=wxa[:, k, :], rhs=hT[b][:, k, :], start=(k == 0), stop=(k == 7)), reads=["wxa", ("hT", b)], writes=["px"])
        P.op("act", lambda e, c=c: e.activation(out=A[:, off(c):off(c) + LRU_CH], in_=px[:, 0:LRU_CH], func=AF.Copy), reads=["px"], writes=["A"])
    L_ = W_ - 4
    for p0 in range(0, L_, PSZ):
        n = min(PSZ, L_ - p0)
        zs = slice(2 + p0, 2 + p0 + n)
        P.op("dve", lambda e, p0=p0, n=n, zs=zs: e.scalar_tensor_tensor(out=B[:, zs], in0=A[:, p0:p0 + n], scalar=cw[:, 0:1], in1=ones[:, 0:n], op0=ALU.mult, op1=ALU.mult), reads=["A", "cw", "ones"], writes=["B"])
        for tap in range(1, 4):
            P.op("dve", lambda e, tap=tap, p0=p0, n=n, zs=zs: e.scalar_tensor_tensor(out=B[:, zs], in0=A[:, tap + p0:tap + p0 + n], scalar=cw[:, tap:tap + 1], in1=B[:, zs], op0=ALU.mult, op1=ALU.add), reads=["A", "cw", "B"], writes=["B"])
        P.op("dve", lambda e, n=n, zs=zs: e.scalar_tensor_tensor(out=B[:, zs], in0=ones[:, 0:n], scalar=cb[:, 0:1], in1=B[:, zs], op0=ALU.mult, op1=ALU.add), reads=["ones", "cb", "B"], writes=["B"])
    pr = P.ps([128, 512], name="L_pr"); pi_ = P.ps([128, 512], name="L_pi")
    zbc = P.sb([128, LRU_CH], BF16, name="L_zbc")
    r = P.sb([128, LRU_CH], name="L_r"); ig = P.sb([128, LRU_CH], name="L_i"); a = P.sb([128, LRU_CH], name="L_a"); a2 = P.sb([128, LRU_CH], name="L_a2")
    om = P.sb([128, LRU_CH], name="L_om"); sq = P.sb([128, LRU_CH], name="L_sq"); t1 = P.sb([128, LRU_CH], name="L_t1"); bt = P.sb([128, LRU_CH], name="L_bt")

    def coeffs(c, dr):
        zs = slice(off(c), off(c) + LRU_CH)
        P.op("act", lambda e: e.activation(out=zbc[:], in_=B[:, zs], func=AF.Copy), reads=["B"], writes=["zbc"])
        P.op("pe", lambda e: e.matmul(pr[:, 0:LRU_CH], lhsT=wa[:, dr, :], rhs=zbc[:], start=True, stop=True), reads=["wa", "zbc"], writes=["pr"])
        P.op("pe", lambda e: e.matmul(pi_[:, 0:LRU_CH], lhsT=wx[:, dr, :], rhs=zbc[:], start=True, stop=True), reads=["wx", "zbc"], writes=["pi"])
        P.op("act", lambda e: e.activation(out=r[:], in_=pr[:, 0:LRU_CH], func=AF.Sigmoid, bias=ba[:, dr:dr + 1]), reads=["pr", "ba"], writes=["r"])
        P.op("act", lambda e: e.activation(out=ig[:], in_=pi_[:, 0:LRU_CH], func=AF.Sigmoid, bias=bx[:, dr:dr + 1]), reads=["pi", "bx"], writes=["ig"])
        P.op("act", lambda e: e.activation(out=a[:], in_=r[:], func=AF.Exp, scale=sdec[:, dr:dr + 1]), reads=["r", "sdec"], writes=["a"])
        P.op("act", lambda e: e.activation(out=a2[:], in_=a[:], func=AF.Square), reads=["a"], writes=["a2"])
        P.op("dve", lambda e: e.scalar_tensor_tensor(out=om[:], in0=a2[:], scalar=-1.0, in1=ones[:, 0:LRU_CH], op0=ALU.mult, op1=ALU.add), reads=["a2", "ones"], writes=["om"])
        P.op("act", lambda e: e.activation(out=sq[:], in_=om[:], func=AF.Sqrt), reads=["om"], writes=["sq"])
        P.op("dve", lambda e: e.tensor_tensor(out=t1[:], in0=ig[:], in1=B[:, zs], op=ALU.mult), reads=["ig", "B"], writes=["t1"])
        P.op("dve", lambda e: e.tensor_tensor(out=bt[:], in0=sq[:], in1=t1[:], op=ALU.mult), reads=["sq", "t1"], writes=["bt"])

    prev = None
    for c in range(NCH):
        coeffs(c, 0)
        ys = slice(off(c), off(c) + LRU_CH)
        init = 0.0 if prev is None else A[:, prev:prev + 1]
        P.op("dve", lambda e, ys=ys, init=init: e.tensor_tensor_scan(out=A[:, ys], data0=a[:], data1=bt[:], initial=init, op0=ALU.mult, op1=ALU.add), reads=["a", "bt", "A"], writes=["A"])
        prev = off(c) + LRU_CH - 1
    yrc = P.sb([128, LRU_CH], name="L_yrc"); st = P.sb([128, 1], name="L_st"); ysum = P.sb([128, LRU_CH], name="L_ys")
    sga = P.sb([128, LRU_CH], name="L_sga"); mo = [P.sb([128, LRU_CH], BF16, name=f"L_mo{i}") for i in range(2)]
    first = True
    for c in [0] + list(range(NCH - 1, 0, -1)):
        coeffs(c, 1)
        init = 0.0 if first else st[:, 0:1]
        P.op("dve", lambda e, init=init: e.tensor_tensor_scan(out=yrc[:, ::-1], data0=a[:, ::-1], data1=bt[:, ::-1], initial=init, op0=ALU.mult, op1=ALU.add),
             reads=["a", "bt", "st"], writes=["yrc"])
        P.op("dve", lambda e: e.tensor_copy(out=st[:], in_=yrc[:, 0:1]), reads=["yrc"], writes=["st"])
        first = False
        P.op("dve", lambda e, c=c: e.tensor_tensor(out=ysum[:], in0=yrc[:], in1=A[:, off(c):off(c) + LRU_CH], op=ALU.add), reads=["yrc", "A"], writes=["ysum"])
        b = load_h(c)
        for k in range(8):
            P.op("pe", lambda e, k=k, b=b: e.matmul(pgp[:, 0:LRU_CH], lhsT=wga[:, k, :], rhs=hT[b][:, k, :], start=(k == 0), stop=(k == 7)), reads=["wga", ("hT", b)], writes=["pg"])
        P.op("act", lambda e: e.activation(out=sga[:], in_=pgp[:, 0:LRU_CH], func=AF.Silu), reads=["pg"], writes=["sga"])
        mb = c % 2
        P.op("dve", lambda e, mb=mb: e.tensor_tensor(out=mo[mb][:], in0=ysum[:], in1=sga[:], op=ALU.mult), reads=["ysum", "sga"], writes=[("mo", mb)])
        P.dma("pool", out_d[:, c * LRU_CH:(c + 1) * LRU_CH], mo[mb][:], reads=[("mo", mb)], chan=f"L_out{mb}")
    P.emit()
    return nc


N_CORES = 8
SEQ, CTX_LEN, DEPTH = 16384, 256, 4
LAT_PER_CORE = SEQ // 4
CTX_PER_CORE = CTX_LEN // 4
NT_FULL = LAT_PER_CORE // 128 + 1

IMPLEMENTED_STAGES = (
    "T (build_T / run_T; head-only, tail+head and tail-only programs): token-parallel out-proj/post-norm/gated-residual + adaLN "
    "pre-norm -- device-verified at full size",
    "M_odd (build_Modd(need_ctx=True) / run_M_odd): retention head (RoPE, bidirectional chunk recurrence, ctx->latent "
    "state, head norm, SiLU gate) -- device-verified at FULL scale, fed with the CPU oracle's layer inputs: "
    "layer 1 -> x1 resid-var 1.2e-5 (layer update alone 5.5e-5), ctx stream 1.0e-5, h2 1.2e-5; "
    "layer 3 -> final x vs the staged expected output 9.0e-6 (layer update alone 5.9e-5)",
)
MISSING_STAGES = (
    "M_even (layers 0 and 2): assembled only as host glue run_M_even over the standalone programs (build_LRU_full x2 launches, "
    "build_FFT(128), build_FFT(2)), each device-verified at full length on the oracle's h0 -- but run_M_even itself and the full "
    "kernel() chain have NEVER been run end to end (plumbing checked on CPU with a fake runner only)",
    "the unfused chain needs >30 min of launches on the development setup: over kernel()'s 1200 s watchdog -> no validate PASS; "
    "programs must be merged / launches made cheaper before it can be banked",
    "build_Modd(need_ctx=False) -- sim-clean but HANGS on device (cause not isolated); unused: run_M_odd always runs "
    "the verified need_ctx=True program and the last layer ignores the context rows",
)


def _rep(v, n):
    return np.ascontiguousarray(np.broadcast_to(np.asarray(v, np.float32)[None, :], (128, n)))


def _cond_cols(c_b, c_ctx):
    return np.ascontiguousarray(np.concatenate([c_b.reshape(8, 128).T, c_ctx.reshape(8, 128).T], axis=1).astype(np.float32))


def run_T_head(x, ctx, c, c_ctx, mod_w_l, mod_b_l, pre_g_l):
    nc = build_T(NT_FULL, 1, False, True)
    in_maps = []
    for core in range(N_CORES):
        b, j = core // 4, core % 4
        xr = np.zeros((NT_FULL * 128, D), np.float32)
        xr[:LAT_PER_CORE] = x[b, j * LAT_PER_CORE:(j + 1) * LAT_PER_CORE]
        xr[LAT_PER_CORE:LAT_PER_CORE + CTX_PER_CORE] = ctx[b, j * CTX_PER_CORE:(j + 1) * CTX_PER_CORE]
        in_maps.append({"x": xr, "cv": _cond_cols(c[b], c_ctx),
                        "modw_c": np.ascontiguousarray(mod_w_l[:, 0:2 * D]), "modb_c": _rep(mod_b_l[0:2 * D], 2 * D),
                        "preg": _rep(pre_g_l, D)})
    res = run_bass_kernel_spmd(nc, in_maps, core_ids=list(range(N_CORES)))
    h = np.empty((2, SEQ, D), NPBF); hc = np.empty((2, CTX_LEN, D), NPBF)
    for core in range(N_CORES):
        b, j = core // 4, core % 4
        o = np.asarray(res.results[core]["h"])
        h[b, j * LAT_PER_CORE:(j + 1) * LAT_PER_CORE] = o[:LAT_PER_CORE]
        hc[b, j * CTX_PER_CORE:(j + 1) * CTX_PER_CORE] = o[LAT_PER_CORE:LAT_PER_CORE + CTX_PER_CORE]
    return h, hc


def _ret_tables(n_lat):
    nch = 2 + n_lat
    tl = n_lat * 128
    pos = np.arange(tl); row = pos // 64; col = pos % 64
    inv = 10000.0 ** (-np.arange(64) / 64.0)
    cosT = np.ones((nch, 128, 2, 128), np.float32); sinT = np.zeros((nch, 128, 2, 128), np.float32)
    for a, p_ax in enumerate((row, col)):
        ang = p_ax[None, :] * inv[np.arange(128) % 64][:, None]
        sgn = np.where(np.arange(128) < 64, -1.0, 1.0)[:, None]
        cosT[2:, :, a, :] = np.cos(ang).reshape(128, n_lat, 128).transpose(1, 0, 2)
        sinT[2:, :, a, :] = (sgn * np.sin(ang)).reshape(128, n_lat, 128).transpose(1, 0, 2)
    jj = np.arange(128)[:, None]; ii = np.arange(128)[None, :]
    expo = np.zeros((128, 2, 128), np.float32); mask = np.zeros((128, 2, 128), np.float32)
    mask[:, 0, :] = (jj <= ii); expo[:, 0, :] = np.where(jj <= ii, ii - jj, 0)
    mask[:, 1, :] = (jj > ii); expo[:, 1, :] = np.where(jj > ii, jj - ii, 0)
    idx = np.arange(128, dtype=np.float32)
    vexp = np.stack([idx + 1, 128 - idx, 127 - idx, idx], axis=1).astype(np.float32)
    ident = np.eye(128, dtype=np.float32).astype(NPBF)
    return cosT, sinT, expo, mask, vexp, ident


_ROPE_PERM = np.concatenate([np.arange(64, 128), np.arange(0, 64), np.arange(192, 256), np.arange(128, 192)])


def run_M_odd(h, hc, w_in, log_gamma, need_ctx=True):
    n_lat = h.shape[1] // 128
    nch = 2 + n_lat
    cosT, sinT, expo, mask, vexp, ident = _ret_tables(n_lat)
    nc = build_Modd(n_lat, True)
    in_maps = []
    for b in range(2):
        hseq = np.concatenate([np.asarray(hc[b]), np.asarray(h[b])], axis=0).astype(NPBF)
        hT = np.ascontiguousarray(hseq.reshape(nch, 128, 8, 128).transpose(0, 3, 2, 1))
        for hd in range(4):
            wq = w_in[:, hd * QK:(hd + 1) * QK]; wk = w_in[:, 1024 + hd * QK:1024 + (hd + 1) * QK]
            in_maps.append({"hT": hT, "wq": np.ascontiguousarray(wq), "wqp": np.ascontiguousarray(wq[:, _ROPE_PERM]),
                            "wk": np.ascontiguousarray(wk), "wkp": np.ascontiguousarray(wk[:, _ROPE_PERM]),
                            "wv": np.ascontiguousarray(w_in[:, 2048 + hd * DV:2048 + (hd + 1) * DV]),
                            "wg": np.ascontiguousarray(w_in[:, 4096 + hd * DV:4096 + (hd + 1) * DV]),
                            "cosT": cosT, "sinT": sinT,
                            "lg": np.ascontiguousarray(np.broadcast_to(np.asarray(log_gamma)[:, hd][None, :], (128, 2)).astype(np.float32)),
                            "expo": expo, "mask": mask, "vexp": vexp, "ident": ident})
    res = run_bass_kernel_spmd(nc, in_maps, core_ids=list(range(N_CORES)))
    return [np.concatenate([np.asarray(res.results[b * 4 + hd]["m"]) for hd in range(4)], axis=1) for b in range(2)]


def run_T(x, xc, m, c, c_ctx, w_out, mod_w_prev, mod_b_prev, post_g_prev, mod_w_cur=None, mod_b_cur=None, pre_g_cur=None):
    has_head = mod_w_cur is not None
    nc = build_T(NT_FULL, 1, True, has_head)
    in_maps = []
    for core in range(N_CORES):
        b, j = core // 4, core % 4
        ls = slice(j * LAT_PER_CORE, (j + 1) * LAT_PER_CORE); cs_ = slice(j * CTX_PER_CORE, (j + 1) * CTX_PER_CORE)
        xr = np.zeros((NT_FULL * 128, D), np.float32)
        xr[:LAT_PER_CORE] = x[b, ls]; xr[LAT_PER_CORE:LAT_PER_CORE + CTX_PER_CORE] = xc[b, cs_]
        mr = np.zeros((NT_FULL * 128, 2 * D), NPBF)
        mr[:LAT_PER_CORE] = m[b][CTX_LEN + j * LAT_PER_CORE:CTX_LEN + (j + 1) * LAT_PER_CORE]
        mr[LAT_PER_CORE:LAT_PER_CORE + CTX_PER_CORE] = m[b][cs_]
        im = {"x": xr, "cv": _cond_cols(c[b], c_ctx),
              "mT": np.ascontiguousarray(mr.reshape(NT_FULL, 128, 16, 128).transpose(0, 3, 2, 1)),
              "w_out": np.ascontiguousarray(w_out),
              "modw_p": np.ascontiguousarray(mod_w_prev[:, 2 * D:3 * D]), "modb_p": _rep(mod_b_prev[2 * D:3 * D], D),
              "postg": _rep(post_g_prev, D)}
        if has_head:
            im.update({"modw_c": np.ascontiguousarray(mod_w_cur[:, 0:2 * D]), "modb_c": _rep(mod_b_cur[0:2 * D], 2 * D),
                       "preg": _rep(pre_g_cur, D)})
        in_maps.append(im)
    res = run_bass_kernel_spmd(nc, in_maps, core_ids=list(range(N_CORES)))
    x_new = np.empty((2, SEQ, D), np.float32); xc_new = np.empty((2, CTX_LEN, D), np.float32)
    h = np.empty((2, SEQ, D), NPBF) if has_head else None
    hc = np.empty((2, CTX_LEN, D), NPBF) if has_head else None
    for core in range(N_CORES):
        b, j = core // 4, core % 4
        ls = slice(j * LAT_PER_CORE, (j + 1) * LAT_PER_CORE); cs_ = slice(j * CTX_PER_CORE, (j + 1) * CTX_PER_CORE)
        xo = np.asarray(res.results[core]["xo"])
        x_new[b, ls] = xo[:LAT_PER_CORE]; xc_new[b, cs_] = xo[LAT_PER_CORE:LAT_PER_CORE + CTX_PER_CORE]
        if has_head:
            ho = np.asarray(res.results[core]["h"])
            h[b, ls] = ho[:LAT_PER_CORE]; hc[b, cs_] = ho[LAT_PER_CORE:LAT_PER_CORE + CTX_PER_CORE]
    return x_new, xc_new, h, hc


def run_M_even(h, hc, w_in, conv_w, conv_b, wa, ba, wx, bx, lam, w_f):
    f32 = lambda a_: np.ascontiguousarray(a_, np.float32)
    n_lat = SEQ // LRU_CH
    nch = 1 + n_lat
    m = [np.empty((CTX_LEN + SEQ, 2 * D), NPBF) for _ in range(2)]
    hseq = [np.concatenate([np.asarray(hc[b]), np.asarray(h[b])], axis=0).astype(NPBF) for b in range(2)]
    hT = [np.ascontiguousarray(hs.reshape(nch, LRU_CH, 8, 128).transpose(0, 3, 2, 1)) for hs in hseq]
    nc = build_LRU_full(n_lat)
    for rnd in range(2):
        b = rnd
        in_maps = []
        for hd in range(8):
            cols = slice(hd * 128, (hd + 1) * 128)
            in_maps.append({"hT": hT[b], "wxa": f32(w_in[:, 0:D][:, cols]), "wga": f32(w_in[:, 2 * D:3 * D][:, cols]),
                            "convw": f32(conv_w[:, cols].T), "convb": f32(conv_b[cols][:, None]),
                            "wa": f32(wa[:, hd].transpose(1, 0, 2)), "wx": f32(wx[:, hd].transpose(1, 0, 2)),
                            "ba": f32(ba[:, cols].T), "bx": f32(bx[:, cols].T), "lam": f32(lam[:, cols].T)})
        res = run_bass_kernel_spmd(nc, in_maps, core_ids=list(range(N_CORES)))
        for hd in range(8):
            m[b][:, hd * 128:(hd + 1) * 128] = np.asarray(res.results[hd]["mA"]).T
    for (n2, src, r0, r1) in ((SEQ // 128, h, CTX_LEN, CTX_LEN + SEQ), (CTX_LEN // 128, hc, 0, CTX_LEN)):
        ncf = build_FFT(n2)
        in_maps = []
        for core in range(N_CORES):
            b, g = core // 4, core % 4
            in_maps.append(fft_host_inputs(np.asarray(src[b]), w_in[:, D + g * FFT_GC:D + (g + 1) * FFT_GC],
                                           w_in[:, 3 * D + g * FFT_GC:3 * D + (g + 1) * FFT_GC], w_f[g]))
        res = run_bass_kernel_spmd(ncf, in_maps, core_ids=list(range(N_CORES)))
        for core in range(N_CORES):
            b, g = core // 4, core % 4
            m[b][r0:r1, D + g * FFT_GC:D + (g + 1) * FFT_GC] = np.asarray(res.results[core]["yB"]).reshape(r1 - r0, FFT_GC)
    return m


def kernel(x, c, ctx, c_ctx, mod_w, mod_b, pre_g, post_g, mix_w_in, mix_w_out, conv_w, conv_b,
           lru_wa, lru_ba, lru_wx, lru_bx, lru_lam, fnet_w, ret_w_in, ret_w_out, ret_log_gamma):
    import time as _t
    t0 = _t.time()
    a_ = lambda v: np.asarray(v, np.float32)
    x = a_(x); xc = a_(ctx); c = a_(c); c_ctx = a_(c_ctx)
    mod_w, mod_b, pre_g, post_g = a_(mod_w), a_(mod_b), a_(pre_g), a_(post_g)
    h, hc = run_T_head(x, xc, c, c_ctx, mod_w[0], mod_b[0], pre_g[0])
    print("[kernel] T head done %.0f s" % (_t.time() - t0), flush=True)
    for L in range(DEPTH):
        if L % 2 == 0:
            e = L // 2
            m = run_M_even(h, hc, a_(mix_w_in[e]), a_(conv_w[e]), a_(conv_b[e]), a_(lru_wa[e]), a_(lru_ba[e]), a_(lru_wx[e]),
                           a_(lru_bx[e]), a_(lru_lam[e]), a_(fnet_w[e]))
            w_out = a_(mix_w_out[e])
        else:
            j = L // 2
            m = run_M_odd(h, hc, a_(ret_w_in[j]), a_(ret_log_gamma[j]))
            w_out = a_(ret_w_out[j])
        print("[kernel] layer %d mixer done %.0f s" % (L, _t.time() - t0), flush=True)
        if L < DEPTH - 1:
            x, xc, h, hc = run_T(x, xc, m, c, c_ctx, w_out, mod_w[L], mod_b[L], post_g[L], mod_w[L + 1], mod_b[L + 1], pre_g[L + 1])
        else:
            x, _, _, _ = run_T(x, xc, m, c, c_ctx, w_out, mod_w[L], mod_b[L], post_g[L])
        print("[kernel] layer %d done %.0f s" % (L, _t.time() - t0), flush=True)
    return x
```
